# Optimizing a Trainium2 kernel written in Bass

```python
import jax
import jax.numpy as jnp
from jax import lax
import numpy as np

D_MODEL = 2048
BATCH = 4
SEQ = 8192
DEPTH = 2
DEC_BATCH = 1
DEC_SEQ = 16384
PAST_LEN = 128

GRID_W = 64
ROPE_THETA = 10000.0
Q_BLOCK = 128
LN_EPS = 1e-5
RMS_EPS = 1e-6
DEEPNORM_ALPHA = (2 * DEPTH) ** 0.25
DEEPNORM_BETA = (8 * DEPTH) ** -0.25

A_HEAD_DIM = 128
A_Q_HEADS = (D_MODEL // 2) // A_HEAD_DIM
A_KV_HEADS = A_Q_HEADS // 4
A_GROUP = A_Q_HEADS // A_KV_HEADS
B_HEADS = 4
B_HEAD_DIM = (D_MODEL // 2) // B_HEADS
MLSTM_CHUNK = 64
MLSTM_FGATE_BIAS = 3.0
C_HEADS = 4
C_KEY_DIM = (D_MODEL // 4) // C_HEADS
C_VAL_DIM = (D_MODEL // 2) // C_HEADS
GLA_GATE_RANK = 16
GLA_TAU = 16.0
GLA_CHUNK = 64
D_HEADS = 8
D_NOPE_DIM = 128
D_ROPE_DIM = 64
D_V_DIM = (D_MODEL // 2) // D_HEADS
D_Q_RANK = D_MODEL // 4
D_KV_RANK = D_MODEL // 8
N_GROUPS = 4
EXPERTS_PER_GROUP = 8
N_EXPERTS = N_GROUPS * EXPERTS_PER_GROUP
TOP_K = 2
D_EXPERT = D_MODEL // 4
MOE_BLOCK = 256

N_EVEN = (DEPTH + 1) // 2
N_ODD = DEPTH // 2

AB_WIDTHS = (A_Q_HEADS * A_HEAD_DIM, A_KV_HEADS * A_HEAD_DIM, A_KV_HEADS * A_HEAD_DIM,
             B_HEADS * B_HEAD_DIM, B_HEADS * B_HEAD_DIM, B_HEADS * B_HEAD_DIM, B_HEADS * B_HEAD_DIM,
             4 * B_HEADS)
AB_IN = sum(AB_WIDTHS)
AB_MIX = A_Q_HEADS * A_HEAD_DIM + B_HEADS * B_HEAD_DIM
CD_WIDTHS = (C_HEADS * C_KEY_DIM, C_HEADS * C_KEY_DIM, C_HEADS * C_VAL_DIM, C_HEADS * C_VAL_DIM,
             GLA_GATE_RANK, GLA_GATE_RANK, D_Q_RANK, D_KV_RANK, D_ROPE_DIM)
CD_IN = sum(CD_WIDTHS)
CD_MIX = C_HEADS * C_VAL_DIM + D_HEADS * D_V_DIM

kernel_name = 'hybrid_bidir_gqa_mlstm_gla_mla_hmoe_encoder'


def split_cols(z, widths):
    cuts = [int(v) for v in np.cumsum(widths)[:-1]]
    return jnp.split(z, cuts, axis=-1)


def layer_norm(x, g, b):
    xf = x.astype(jnp.float32)
    xc = xf - jnp.mean(xf, axis=-1, keepdims=True)
    var = jnp.mean(xc * xc, axis=-1, keepdims=True)
    return (xc * lax.rsqrt(var + LN_EPS) * g + b).astype(x.dtype)


def rms_norm(x, g):
    xf = x.astype(jnp.float32)
    return (xf * lax.rsqrt(jnp.mean(xf * xf, axis=-1, keepdims=True) + RMS_EPS) * g).astype(x.dtype)


def grid_positions(n_tokens):
    n_rows = n_tokens // GRID_W
    row = jnp.repeat(jnp.arange(n_rows, dtype=jnp.int32), GRID_W)
    col = jnp.tile(jnp.arange(GRID_W, dtype=jnp.int32), n_rows)
    return row, col


def rope_1d(x, pos):
    m = x.shape[-1]
    inv_freq = ROPE_THETA ** (-jnp.arange(0, m, 2, dtype=jnp.float32) / m)
    ang = pos.astype(jnp.float32)[:, None] * inv_freq[None, :]
    cos = jnp.cos(ang)[:, None, :]
    sin = jnp.sin(ang)[:, None, :]
    x1, x2 = jnp.split(x.astype(jnp.float32), 2, axis=-1)
    return jnp.concatenate([x1 * cos - x2 * sin, x1 * sin + x2 * cos], axis=-1).astype(x.dtype)


def axial_rope(x, row, col):
    half = x.shape[-1] // 2
    return jnp.concatenate([rope_1d(x[..., :half], row), rope_1d(x[..., half:], col)], axis=-1)


def block_attention(q, k, v, scale):
    B, S, Hk, G, dq = q.shape
    dv = v.shape[-1]
    nb = S // Q_BLOCK
    qb = jnp.swapaxes(q.reshape(B, nb, Q_BLOCK, Hk, G, dq), 0, 1)

    def attend(qblk):
        s = jnp.einsum('bqhgd,bkhd->bhgqk', qblk, k).astype(jnp.float32) * scale
        p = jax.nn.softmax(s, axis=-1).astype(v.dtype)
        return jnp.einsum('bhgqk,bkhe->bqhge', p, v)

    o = lax.map(attend, qb)
    return jnp.swapaxes(o, 0, 1).reshape(B, S, Hk * G * dv)


def chunked(a, L):
    B, S, H = a.shape[:3]
    a = a.reshape((B, S // L, L, H) + a.shape[3:])
    return jnp.swapaxes(jnp.swapaxes(a, 2, 3), 0, 1)


def unchunked(a):
    nc, B, H, L, d = a.shape
    return jnp.swapaxes(jnp.swapaxes(a, 0, 1), 2, 3).reshape(B, nc * L, H, d)


def mlstm_chunked(q, k, v, i_pre, f_pre):
    B, S, H, dk = q.shape
    dv = v.shape[-1]
    L = MLSTM_CHUNK
    causal = jnp.tril(jnp.ones((L, L), dtype=bool))
    qs = chunked(q.astype(jnp.float32) * (dk ** -0.5), L)
    ks = chunked(k.astype(jnp.float32), L)
    vs = chunked(v.astype(jnp.float32), L)
    li = chunked(i_pre.astype(jnp.float32), L)
    lf = chunked(jax.nn.log_sigmoid(f_pre.astype(jnp.float32)), L)

    def step(carry, xs):
        C, n, m = carry
        qc, kc, vc, lic, lfc = xs
        b = jnp.cumsum(lfc, axis=-1)
        logw = jnp.where(causal, b[..., :, None] - b[..., None, :] + lic[..., None, :], -jnp.inf)
        m_inter = b + m[..., None]
        m_t = jnp.maximum(jnp.max(logw, axis=-1), m_inter)
        w = jnp.exp(logw - m_t[..., None]) * jnp.einsum('bhtd,bhsd->bhts', qc, kc)
        s_inter = jnp.exp(m_inter - m_t)
        num = jnp.einsum('bhts,bhse->bhte', w, vc) + s_inter[..., None] * jnp.einsum('bhtd,bhde->bhte', qc, C)
        den = jnp.sum(w, axis=-1) + s_inter * jnp.einsum('bhtd,bhd->bht', qc, n)
        h = num / jnp.maximum(jnp.abs(den), jnp.exp(-m_t))[..., None]
        g = b[..., -1]
        logu = g[..., None] - b + lic
        m_new = jnp.maximum(g + m, jnp.max(logu, axis=-1))
        decay = jnp.exp(g + m - m_new)
        u = jnp.exp(logu - m_new[..., None])
        C = decay[..., None, None] * C + jnp.einsum('bhs,bhsd,bhse->bhde', u, kc, vc)
        n = decay[..., None] * n + jnp.einsum('bhs,bhsd->bhd', u, kc)
        return (C, n, m_new), h

    init = (jnp.zeros((B, H, dk, dv), jnp.float32), jnp.zeros((B, H, dk), jnp.float32),
            jnp.zeros((B, H), jnp.float32))
    _, hs = lax.scan(step, init, (qs, ks, vs, li, lf))
    return unchunked(hs)


def gla_chunked(q, k, v, log_a):
    B, S, H, dk = q.shape
    dv = v.shape[-1]
    L = GLA_CHUNK
    causal = jnp.tril(jnp.ones((L, L), dtype=bool))[..., None]
    qs = chunked(q.astype(jnp.float32) * (dk ** -0.5), L)
    ks = chunked(k.astype(jnp.float32), L)
    vs = chunked(v.astype(jnp.float32), L)
    las = chunked(log_a.astype(jnp.float32), L)

    def step(state, xs):
        qc, kc, vc, ac = xs
        G = jnp.cumsum(ac, axis=2)
        diff = jnp.where(causal, G[:, :, :, None, :] - G[:, :, None, :, :], -jnp.inf)
        A = jnp.einsum('bhtd,bhsd,bhtsd->bhts', qc, kc, jnp.exp(diff))
        o = jnp.einsum('bhts,bhse->bhte', A, vc) + jnp.einsum('bhtd,bhde->bhte', qc * jnp.exp(G), state)
        G_last = G[:, :, -1:, :]
        state = jnp.exp(G_last[:, :, 0, :])[..., None] * state + jnp.einsum('bhsd,bhse->bhde', kc * jnp.exp(G_last - G), vc)
        return state, o

    _, os_ = lax.scan(step, jnp.zeros((B, H, dk, dv), jnp.float32), (qs, ks, vs, las))
    return unchunked(os_)


def flip(a):
    return jnp.flip(a, axis=1)


def mixer_ab(u, row, col, w_in, b_in, g_q, g_k, g_mlstm, w_out):
    B, S, _ = u.shape
    z = u @ w_in + b_in
    qa, ka, va, qb, kb, vb, ob, gates = split_cols(z, AB_WIDTHS)
    qa = axial_rope(rms_norm(qa.reshape(B, S, A_Q_HEADS, A_HEAD_DIM), g_q), row, col)
    qa = qa.reshape(B, S, A_KV_HEADS, A_GROUP, A_HEAD_DIM)
    ka = axial_rope(rms_norm(ka.reshape(B, S, A_KV_HEADS, A_HEAD_DIM), g_k), row, col)
    va = va.reshape(B, S, A_KV_HEADS, A_HEAD_DIM)
    out_a = block_attention(qa, ka, va, A_HEAD_DIM ** -0.5)
    qb = qb.reshape(B, S, B_HEADS, B_HEAD_DIM)
    kb = kb.reshape(B, S, B_HEADS, B_HEAD_DIM)
    vb = vb.reshape(B, S, B_HEADS, B_HEAD_DIM)
    i_f, f_f, i_b, f_b = jnp.split(gates, 4, axis=-1)
    h_f = mlstm_chunked(qb, kb, vb, i_f, f_f)
    h_b = flip(mlstm_chunked(flip(qb), flip(kb), flip(vb), flip(i_b), flip(f_b)))
    o_gate = jax.nn.sigmoid(ob.astype(jnp.float32)).reshape(B, S, B_HEADS, B_HEAD_DIM)
    out_b = rms_norm(o_gate * (h_f + h_b), g_mlstm.reshape(B_HEADS, B_HEAD_DIM))
    out_b = out_b.reshape(B, S, B_HEADS * B_HEAD_DIM).astype(u.dtype)
    return jnp.concatenate([out_a, out_b], axis=-1) @ w_out


def mixer_cd(u, row, col, w_in, b_in, w_gla_f, b_gla_f, w_gla_b, b_gla_b, g_gla,
             g_cq, w_uq, g_ckv, w_ukv, w_out):
    B, S, _ = u.shape
    z = u @ w_in + b_in
    qc, kc, vc, rc, lr_f, lr_b, cq, ckv, kr = split_cols(z, CD_WIDTHS)
    qc = qc.reshape(B, S, C_HEADS, C_KEY_DIM)
    kc = kc.reshape(B, S, C_HEADS, C_KEY_DIM)
    vc = vc.reshape(B, S, C_HEADS, C_VAL_DIM)
    la_f = (jax.nn.log_sigmoid((lr_f @ w_gla_f + b_gla_f).astype(jnp.float32)) / GLA_TAU).reshape(B, S, C_HEADS, C_KEY_DIM)
    la_b = (jax.nn.log_sigmoid((lr_b @ w_gla_b + b_gla_b).astype(jnp.float32)) / GLA_TAU).reshape(B, S, C_HEADS, C_KEY_DIM)
    o_f = gla_chunked(qc, kc, vc, la_f)
    o_b = flip(gla_chunked(flip(qc), flip(kc), flip(vc), flip(la_b)))
    out_c = rms_norm(o_f + o_b, g_gla.reshape(C_HEADS, C_VAL_DIM)).reshape(B, S, C_HEADS * C_VAL_DIM)
    out_c = (out_c * jax.nn.silu(rc.astype(jnp.float32))).astype(u.dtype)
    q = (rms_norm(cq, g_cq) @ w_uq).reshape(B, S, D_HEADS, D_NOPE_DIM + D_ROPE_DIM)
    q = jnp.concatenate([q[..., :D_NOPE_DIM], axial_rope(q[..., D_NOPE_DIM:], row, col)], axis=-1)
    kv = (rms_norm(ckv, g_ckv) @ w_ukv).reshape(B, S, D_HEADS, D_NOPE_DIM + D_V_DIM)
    k_rope = axial_rope(kr.reshape(B, S, 1, D_ROPE_DIM), row, col)
    k = jnp.concatenate([kv[..., :D_NOPE_DIM], jnp.broadcast_to(k_rope, (B, S, D_HEADS, D_ROPE_DIM))], axis=-1)
    v = kv[..., D_NOPE_DIM:]
    out_d = block_attention(q[:, :, :, None, :], k, v, (D_NOPE_DIM + D_ROPE_DIM) ** -0.5)
    return jnp.concatenate([out_c, out_d], axis=-1) @ w_out


def hier_moe(u, w_rg, b_rg, w_re, b_re, w_gate, w_up, w_down):
    B, S, D = u.shape
    T = B * S
    t = u.reshape(T, D)
    p_grp = jax.nn.softmax((t @ w_rg + b_rg).astype(jnp.float32), axis=-1)
    p_top, g_idx = lax.top_k(p_grp, 1)
    logit_e = (t @ w_re + b_re).astype(jnp.float32).reshape(T, N_GROUPS, EXPERTS_PER_GROUP)
    logit_in = jnp.take_along_axis(logit_e, g_idx[:, :, None], axis=1)[:, 0]
    p_in, e_local = lax.top_k(jax.nn.softmax(logit_in, axis=-1), TOP_K)
    gate = p_top * p_in / jnp.sum(p_in, axis=-1, keepdims=True)
    expert = g_idx * EXPERTS_PER_GROUP + e_local
    M = T * TOP_K
    flat_e = expert.reshape(M)
    flat_tok = jnp.arange(M, dtype=jnp.int32) // TOP_K
    flat_w = gate.reshape(M)
    order = jnp.argsort(flat_e)
    se, stok, sw = flat_e[order], flat_tok[order], flat_w[order]
    counts = jnp.bincount(flat_e, length=N_EXPERTS)
    padded = (counts + MOE_BLOCK - 1) // MOE_BLOCK * MOE_BLOCK
    start = jnp.cumsum(counts) - counts
    pad_end = jnp.cumsum(padded)
    pad_start = pad_end - padded
    dest = pad_start[se] + jnp.arange(M, dtype=jnp.int32) - start[se]
    n_blocks = -(-M // MOE_BLOCK) + N_EXPERTS
    P = n_blocks * MOE_BLOCK
    slot_tok = jnp.zeros((P,), jnp.int32).at[dest].set(stok)
    slot_w = jnp.zeros((P,), jnp.float32).at[dest].set(sw)
    block_e = jnp.minimum(jnp.searchsorted(pad_end, jnp.arange(n_blocks, dtype=jnp.int32) * MOE_BLOCK, side='right'),
                          N_EXPERTS - 1)

    def run(args):
        tok, e, wt = args
        xb = t[tok]
        h = jax.nn.silu(xb @ w_gate[e]) * (xb @ w_up[e])
        return (h @ w_down[e]) * wt[:, None]

    yb = lax.map(run, (slot_tok.reshape(n_blocks, MOE_BLOCK), block_e, slot_w.reshape(n_blocks, MOE_BLOCK)))
    y = jax.ops.segment_sum(yb.reshape(P, D), slot_tok, num_segments=T)
    return y.astype(u.dtype).reshape(B, S, D)


def trunk(x, c, p):
    row, col = grid_positions(x.shape[1])
    c_act = jax.nn.silu(c)
    for layer in range(DEPTH):
        mod = (c_act @ p['ada_w'][layer] + p['ada_b'][layer])[:, None, :]
        sh1, sc1, g1, sh2, sc2, g2 = jnp.split(mod, 6, axis=-1)
        u = x * (1.0 + sc1) + sh1
        if layer % 2 == 0:
            j = layer // 2
            f = mixer_ab(u, row, col, p['ab_w_in'][j], p['ab_b_in'][j], p['ab_g_q'][j], p['ab_g_k'][j],
                         p['ab_g_mlstm'][j], p['ab_w_out'][j])
        else:
            j = layer // 2
            f = mixer_cd(u, row, col, p['cd_w_in'][j], p['cd_b_in'][j], p['cd_w_gla_f'][j], p['cd_b_gla_f'][j],
                         p['cd_w_gla_b'][j], p['cd_b_gla_b'][j], p['cd_g_gla'][j], p['cd_g_cq'][j],
                         p['cd_w_uq'][j], p['cd_g_ckv'][j], p['cd_w_ukv'][j], p['cd_w_out'][j])
        x = layer_norm(DEEPNORM_ALPHA * x + (1.0 + g1) * f, p['ln1_g'][layer], p['ln1_b'][layer])
        u = x * (1.0 + sc2) + sh2
        f = hier_moe(u, p['moe_w_rg'][layer], p['moe_b_rg'][layer], p['moe_w_re'][layer], p['moe_b_re'][layer],
                     p['moe_w_gate'][layer], p['moe_w_up'][layer], p['moe_w_down'][layer])
        x = layer_norm(DEEPNORM_ALPHA * x + (1.0 + g2) * f, p['ln2_g'][layer], p['ln2_b'][layer])
    return x


def setup_inputs(seed: int = 0) -> dict:
    key = jax.random.key(seed)
    ks = iter(jax.random.split(key, 48))
    D = D_MODEL

    def nrm(shape, scale):
        return scale * jax.random.normal(next(ks), shape, jnp.float32)

    def gain(shape):
        return 1.0 + nrm(shape, 0.02)

    gate_start = AB_IN - 4 * B_HEADS
    ab_b_in = nrm((N_EVEN, AB_IN), 0.02)
    ab_b_in = ab_b_in.at[:, gate_start + B_HEADS:gate_start + 2 * B_HEADS].add(MLSTM_FGATE_BIAS)
    ab_b_in = ab_b_in.at[:, gate_start + 3 * B_HEADS:gate_start + 4 * B_HEADS].add(MLSTM_FGATE_BIAS)
    return {
        'x_prompt': nrm((BATCH, SEQ, D), 1.0),
        'x_sample': nrm((DEC_BATCH, DEC_SEQ, D), 1.0),
        'c_prompt': nrm((BATCH, D), 1.0),
        'c_sample': nrm((DEC_BATCH, D), 1.0),
        'ada_w': nrm((DEPTH, D, 6 * D), 0.5 * D ** -0.5),
        'ada_b': nrm((DEPTH, 6 * D), 0.02),
        'ln1_g': gain((DEPTH, D)),
        'ln1_b': nrm((DEPTH, D), 0.02),
        'ln2_g': gain((DEPTH, D)),
        'ln2_b': nrm((DEPTH, D), 0.02),
        'ab_w_in': nrm((N_EVEN, D, AB_IN), D ** -0.5),
        'ab_b_in': ab_b_in,
        'ab_g_q': gain((N_EVEN, A_HEAD_DIM)),
        'ab_g_k': gain((N_EVEN, A_HEAD_DIM)),
        'ab_g_mlstm': gain((N_EVEN, B_HEADS * B_HEAD_DIM)),
        'ab_w_out': nrm((N_EVEN, AB_MIX, D), DEEPNORM_BETA * AB_MIX ** -0.5),
        'cd_w_in': nrm((N_ODD, D, CD_IN), D ** -0.5),
        'cd_b_in': nrm((N_ODD, CD_IN), 0.02),
        'cd_w_gla_f': nrm((N_ODD, GLA_GATE_RANK, C_HEADS * C_KEY_DIM), GLA_GATE_RANK ** -0.5),
        'cd_b_gla_f': nrm((N_ODD, C_HEADS * C_KEY_DIM), 0.02),
        'cd_w_gla_b': nrm((N_ODD, GLA_GATE_RANK, C_HEADS * C_KEY_DIM), GLA_GATE_RANK ** -0.5),
        'cd_b_gla_b': nrm((N_ODD, C_HEADS * C_KEY_DIM), 0.02),
        'cd_g_gla': gain((N_ODD, C_HEADS * C_VAL_DIM)),
        'cd_g_cq': gain((N_ODD, D_Q_RANK)),
        'cd_w_uq': nrm((N_ODD, D_Q_RANK, D_HEADS * (D_NOPE_DIM + D_ROPE_DIM)), D_Q_RANK ** -0.5),
        'cd_g_ckv': gain((N_ODD, D_KV_RANK)),
        'cd_w_ukv': nrm((N_ODD, D_KV_RANK, D_HEADS * (D_NOPE_DIM + D_V_DIM)), D_KV_RANK ** -0.5),
        'cd_w_out': nrm((N_ODD, CD_MIX, D), DEEPNORM_BETA * CD_MIX ** -0.5),
        'moe_w_rg': nrm((DEPTH, D, N_GROUPS), D ** -0.5),
        'moe_b_rg': nrm((DEPTH, N_GROUPS), 0.01),
        'moe_w_re': nrm((DEPTH, D, N_EXPERTS), D ** -0.5),
        'moe_b_re': nrm((DEPTH, N_EXPERTS), 0.01),
        'moe_w_gate': nrm((DEPTH, N_EXPERTS, D, D_EXPERT), D ** -0.5),
        'moe_w_up': nrm((DEPTH, N_EXPERTS, D, D_EXPERT), D ** -0.5),
        'moe_w_down': nrm((DEPTH, N_EXPERTS, D_EXPERT, D), DEEPNORM_BETA * D_EXPERT ** -0.5),
    }


def reference(x_prompt, x_sample, c_prompt, c_sample, ada_w, ada_b, ln1_g, ln1_b, ln2_g, ln2_b,
              ab_w_in, ab_b_in, ab_g_q, ab_g_k, ab_g_mlstm, ab_w_out,
              cd_w_in, cd_b_in, cd_w_gla_f, cd_b_gla_f, cd_w_gla_b, cd_b_gla_b, cd_g_gla,
              cd_g_cq, cd_w_uq, cd_g_ckv, cd_w_ukv, cd_w_out,
              moe_w_rg, moe_b_rg, moe_w_re, moe_b_re, moe_w_gate, moe_w_up, moe_w_down):
    params = dict(ada_w=ada_w, ada_b=ada_b, ln1_g=ln1_g, ln1_b=ln1_b, ln2_g=ln2_g, ln2_b=ln2_b,
                  ab_w_in=ab_w_in, ab_b_in=ab_b_in, ab_g_q=ab_g_q, ab_g_k=ab_g_k, ab_g_mlstm=ab_g_mlstm,
                  ab_w_out=ab_w_out, cd_w_in=cd_w_in, cd_b_in=cd_b_in, cd_w_gla_f=cd_w_gla_f,
                  cd_b_gla_f=cd_b_gla_f, cd_w_gla_b=cd_w_gla_b, cd_b_gla_b=cd_b_gla_b, cd_g_gla=cd_g_gla,
                  cd_g_cq=cd_g_cq, cd_w_uq=cd_w_uq, cd_g_ckv=cd_g_ckv, cd_w_ukv=cd_w_ukv, cd_w_out=cd_w_out,
                  moe_w_rg=moe_w_rg, moe_b_rg=moe_b_rg, moe_w_re=moe_w_re, moe_b_re=moe_b_re,
                  moe_w_gate=moe_w_gate, moe_w_up=moe_w_up, moe_w_down=moe_w_down)
    y_prompt = trunk(x_prompt, c_prompt, params)
    y_sample = trunk(x_sample, c_sample, params)
    return (y_prompt, y_sample)
```

```python
import math
from contextlib import ExitStack
import numpy as np
import concourse.bass as bass
import concourse.mybir as mybir
from concourse.bass_utils import run_bass_kernel_spmd

F32 = mybir.dt.float32
BF16 = mybir.dt.bfloat16
AF = mybir.ActivationFunctionType
ALU = mybir.AluOpType
AX = mybir.AxisListType

NCORE = 8
D = 2048
KC = 16
TT = 512
DEPTH = 2
ALPHA = (2 * DEPTH) ** 0.25
LN_EPS = 1e-5
RMS_EPS = 1e-6
NEG = -1.0e30


class V:
    __slots__ = ("ap", "keys")

    def __init__(self, ap, *keys):
        self.ap = ap
        self.keys = list(keys)


class Sch:
    EPOCH = 30000
    KRING = 8

    def __init__(self, nc, es):
        self.nc = nc
        self.eng = dict(pe=nc.tensor, act=nc.scalar, dve=nc.vector, pool=nc.gpsimd, sp=nc.sync)
        self.names = list(self.eng)
        self.es = es
        self.sems = {}
        self.ops = {n: [] for n in self.names}
        self.cnt = {n: 0 for n in self.names}
        self.dcnt = {n: 0 for n in self.names}
        self.ccnt = 0
        self.last_w = {}
        self.readers = {}
        self.waited = {n: {} for n in self.names}
        self.last_tok = {n: None for n in self.names}
        self.all_dma = []

    def sem(self, name):
        if name not in self.sems:
            self.sems[name] = self.es.enter_context(self.nc.semaphore(name))
        return self.sems[name]

    def _deps(self, eng, R, W):
        deps = set()
        for v in R:
            for k in v.keys:
                w = self.last_w.get(k)
                if w is not None:
                    deps.add(w)
        for v in W:
            for k in v.keys:
                w = self.last_w.get(k)
                if w is not None:
                    deps.add(w)
                for r in self.readers.get(k, {}).values():
                    if isinstance(r, list):
                        deps.update(r)
                    else:
                        deps.add(r)
        return deps

    def _commit(self, tok, kind, eng, R, W):
        for v in W:
            for k in v.keys:
                self.last_w[k] = tok
                self.readers[k] = {}
        for v in R:
            for k in v.keys:
                d = self.readers.setdefault(k, {})
                if kind == "dma":
                    d.setdefault(("dma", eng), []).append(tok)
                    if len(d[("dma", eng)]) > 2 * self.KRING:
                        d[("dma", eng)] = d[("dma", eng)][-2 * self.KRING:]
                else:
                    d[eng] = tok

    def op(self, eng, fn, R=(), W=()):
        deps = self._deps(eng, R, W)
        i = self.cnt[eng]
        self.cnt[eng] += 1
        tok = ("c", eng, "e_%s_%d" % (eng, i // self.EPOCH), i % self.EPOCH + 1)
        self.ops[eng].append(("c", fn, deps, tok))
        self._commit(tok, "c", eng, R, W)
        self.last_tok[eng] = tok
        return tok

    def dma(self, eng, fn, R=(), W=()):
        deps = self._deps(eng, R, W)
        j = self.dcnt[eng]
        self.dcnt[eng] += 1
        sname = "d_%s_%d" % (eng, j % self.KRING)
        tok = ("d", eng, sname, 16 * (j // self.KRING + 1))
        if j >= self.KRING:
            deps.add(("d", eng, sname, 16 * (j // self.KRING)))
        self.ops[eng].append(("d", fn, deps, tok))
        self._commit(tok, "dma", eng, R, W)
        self.all_dma.append(tok)
        if len(self.all_dma) > 4 * self.KRING:
            self.all_dma = self.all_dma[-4 * self.KRING:]
        return tok

    def coll(self, fn, R=(), W=()):
        deps = self._deps("pool", R, W)
        self.ccnt += 1
        tok = ("x", "pool", "cc", self.ccnt)
        self.ops["pool"].append(("x", fn, deps, tok))
        self._commit(tok, "dma", "pool", R, W)
        self.all_dma.append(tok)
        return tok

    def barrier(self):
        toks = [t for t in self.last_tok.values() if t is not None] + list(self.all_dma)
        for n in self.names:
            self.ops[n].append(("w", None, set(toks), None))

    def flush(self):
        for n in self.names:
            for (_, _, deps, tok) in self.ops[n]:
                for d in deps:
                    self.sem(d[2])
                if tok is not None:
                    self.sem(tok[2])
        with self.nc.Block() as block:
            for n in self.names:
                ops = self.ops[n]
                if not ops:
                    continue
                dec = dict(pe=block.tensor, act=block.scalar, dve=block.vector, pool=block.gpsimd, sp=block.sync)[n]

                def body(e, n=n, ops=ops):
                    wd = self.waited[n]
                    for (kind, fn, deps, tok) in ops:
                        need = {}
                        for d in deps:
                            if d[0] == "c" and d[1] == n and n == "pe":
                                continue
                            if need.get(d[2], 0) < d[3]:
                                need[d[2]] = d[3]
                        for sn, val in need.items():
                            if wd.get(sn, 0) < val:
                                e.wait_ge(self.sems[sn], val)
                                wd[sn] = val
                        if kind == "w":
                            continue
                        ins = fn(e)
                        if kind == "c":
                            ins.then_inc(self.sems[tok[2]], 1)
                        elif kind == "d":
                            ins.then_inc(self.sems[tok[2]], 16)
                        else:
                            ins.then_inc(self.sems[tok[2]])
                dec(body)
        self.ops = {n: [] for n in self.names}


def seq_info(SP):
    SS = 2 * SP
    lens = [SP, SP, SP, SP, SS]
    base = [0, SP, 2 * SP, 3 * SP, 4 * SP]
    own = [L // NCORE for L in lens]
    obase = [0]
    for o in own[:-1]:
        obase.append(obase[-1] + o)
    return lens, base, own, obase


def build_nc(SP):
    lens, base, own, obase = seq_info(SP)
    TOT = sum(lens)
    TO = sum(own)
    SMAX = lens[4]
    NB_MAX = SMAX // 128
    nc = bass.Bass("TRN2", target_bir_lowering=False)

    def ein(name, shape, dt=F32):
        return nc.dram_tensor(name, list(shape), dt, kind="ExternalInput").ap()

    RS = D // NCORE
    HT = TOT // 2
    xT_sh = ein("xT_sh", [RS, TOT])
    x_own = ein("x_own", [D, TO])
    cT = ein("cT", [128, KC, 5])
    ada_sh = ein("ada_sh", [2, RS, 6 * D])
    ada_bT = ein("ada_bT", [2, 128, 96])
    lnp = ein("lnp", [128, 2, 4, KC])
    w0 = ein("w0", [D, 1156])
    b0fm = ein("b0fm", [128, 8])
    b0tm = ein("b0tm", [1, 1156])
    gqk = ein("gqk", [128, 2])
    gmix0 = ein("gmix0", [128, 8])
    wout0 = ein("wout0", [D, D])
    w1 = ein("w1", [D, 1376])
    b1fm = ein("b1fm", [128, 12])
    b1tm = ein("b1tm", [1, 1376])
    wgla = ein("wgla", [16, 2, 128])
    bgla = ein("bgla", [1, 2, 128])
    gcq = ein("gcq", [128, 6])
    wuq = ein("wuq", [512, 192])
    wukv = ein("wukv", [256, 256])
    gmix1 = ein("gmix1", [128, 8])
    wout1 = ein("wout1", [D, D])
    wr = ein("wr", [2, D, 36])
    br = ein("br", [2, 1, 36])
    wg_sh = ein("wg_sh", [2, 4 * D, 512])
    wu_sh = ein("wu_sh", [2, 4 * D, 512])
    wd_sh = ein("wd_sh", [2, 4 * 512, D])
    cos1 = ein("cos1", [128, SMAX])
    sin1 = ein("sin1", [128, SMAX])
    cos2 = ein("cos2", [64, SMAX])
    sin2 = ein("sin2", [64, SMAX])
    cmat = ein("cmat", [128, 10, 128])
    selm = ein("selm", [32, 32, 128])
    y_own = nc.dram_tensor("y_own", [D, TO], F32, kind="ExternalOutput").ap()

    mix0 = nc.dram_tensor("mix0", [256, TOT], BF16).ap()
    G0 = nc.dram_tensor("G0", [NCORE * 256, TOT], BF16).ap()
    mix1a = nc.dram_tensor("mix1a", [256, TOT], BF16).ap()
    G1a = nc.dram_tensor("G1a", [NCORE * 256, TOT], BF16).ap()
    mix1b = nc.dram_tensor("mix1b", [128, TOT], BF16).ap()
    G1b = nc.dram_tensor("G1b", [NCORE * 128, TOT], BF16).ap()
    XT = [nc.dram_tensor("XT%d" % h, [D, HT], F32).ap() for h in range(2)]
    XTi = [nc.dram_tensor("XTi%d" % h, [RS, HT], F32).ap() for h in range(2)]
    ADA = [nc.dram_tensor("ADA%d" % l, [D, 6 * D], F32).ap() for l in range(2)]
    ADAi = [nc.dram_tensor("ADAi%d" % l, [RS, 6 * D], F32).ap() for l in range(2)]
    WG = [nc.dram_tensor("WG%d" % l, [32 * D, 512], F32).ap() for l in range(2)]
    WU = [nc.dram_tensor("WU%d" % l, [32 * D, 512], F32).ap() for l in range(2)]
    WD = [nc.dram_tensor("WD%d" % l, [32 * 512, D], F32).ap() for l in range(2)]
    WGi = [nc.dram_tensor("WGi%d" % l, [4 * D, 512], F32).ap() for l in range(2)]
    WUi = [nc.dram_tensor("WUi%d" % l, [4 * D, 512], F32).ap() for l in range(2)]
    WDi = [nc.dram_tensor("WDi%d" % l, [4 * 512, D], F32).ap() for l in range(2)]
    u1own = nc.dram_tensor("u1own", [D, TO], BF16).ap()
    GU = nc.dram_tensor("GU", [NCORE * D, TO], BF16).ap()
    x1own = nc.dram_tensor("x1own", [D, TO], F32).ap()

    es_all = ExitStack()
    with es_all:
        S = Sch(nc, es_all)
        pid_holder = {}

        uid = [0]

        def sb(es, name, shape, dt=F32):
            uid[0] += 1
            return es.enter_context(nc.sbuf_tensor("%s_%d" % (name, uid[0]), list(shape), dt))

        def psum(es, name, shape=None, dt=F32):
            uid[0] += 1
            return es.enter_context(nc.psum_tensor("%s_%d" % (name, uid[0]), [128, 512] if dt == F32 else [128, 1024], dt))

        def mm(out, lhsT, rhs, start=True, stop=True):
            S.op("pe", lambda e: e.matmul(out.ap, lhsT=lhsT.ap, rhs=rhs.ap, start=start, stop=stop),
                 R=[lhsT, rhs] + ([] if start else [out]), W=[out])

        def tr(out, in_, ident):
            S.op("pe", lambda e: e.transpose(out.ap, in_.ap, ident.ap), R=[in_, ident], W=[out])

        def act(out, in_, func, bias=0.0, scale=1.0, eng="act"):
            R = [in_]
            if isinstance(bias, V):
                R.append(bias)
            if isinstance(scale, V):
                R.append(scale)
            b = bias.ap if isinstance(bias, V) else bias
            s = scale.ap if isinstance(scale, V) else scale
            S.op(eng, lambda e: e.activation(out=out.ap, in_=in_.ap, func=func, bias=b, scale=s), R=R, W=[out])

        def tt(out, a, b, op, eng="dve"):
            S.op(eng, lambda e: e.tensor_tensor(out=out.ap, in0=a.ap, in1=b.ap, op=op), R=[a, b], W=[out])

        def ts(out, a, s1, op0, s2=None, op1=None, eng="dve"):
            R = [a] + [x for x in (s1, s2) if isinstance(x, V)]
            v1 = s1.ap if isinstance(s1, V) else s1
            v2 = s2.ap if isinstance(s2, V) else s2
            if op1 is None:
                S.op(eng, lambda e: e.tensor_scalar(out=out.ap, in0=a.ap, scalar1=v1, scalar2=None, op0=op0), R=R, W=[out])
            else:
                S.op(eng, lambda e: e.tensor_scalar(out=out.ap, in0=a.ap, scalar1=v1, scalar2=v2, op0=op0, op1=op1),
                     R=R, W=[out])

        def stt(out, a, sc, b, op0, op1, eng="dve"):
            R = [a, b] + ([sc] if isinstance(sc, V) else [])
            v = sc.ap if isinstance(sc, V) else sc
            S.op(eng, lambda e: e.scalar_tensor_tensor(out=out.ap, in0=a.ap, scalar=v, in1=b.ap, op0=op0, op1=op1),
                 R=R, W=[out])

        def cp(out, in_, eng="dve"):
            if eng == "act":
                S.op(eng, lambda e: e.activation(out=out.ap, in_=in_.ap, func=AF.Identity), R=[in_], W=[out])
            else:
                S.op(eng, lambda e: e.tensor_copy(out=out.ap, in_=in_.ap), R=[in_], W=[out])

        def recip(out, in_):
            S.op("dve", lambda e: e.reciprocal(out=out.ap, in_=in_.ap), R=[in_], W=[out])

        def rmax(out, in_):
            S.op("dve", lambda e: e.reduce_max(out=out.ap, in_=in_.ap, axis=AX.X), R=[in_], W=[out])

        def rsum(out, in_):
            S.op("dve", lambda e: e.reduce_sum(out=out.ap, in_=in_.ap, axis=AX.X), R=[in_], W=[out])

        def mset(out, val, eng="pool"):
            S.op(eng, lambda e: e.memset(out.ap, val), W=[out])

        def dma(out, in_, eng="sp"):
            S.dma(eng, lambda e: e.dma_start(out=out.ap, in_=in_.ap), R=[in_], W=[out])

        def dma_dyn(out, mk_in, Rk, eng="pool"):
            def fn(e):
                if "pid" not in pid_holder:
                    pid_holder["pid"] = e.partition_id()
                src = mk_in(pid_holder["pid"])
                try:
                    return e.dma_start(out=out.ap, in_=src)
                except Exception:
                    print("DYN DMA FAIL", S.dcnt, out.ap, src)
                    raise
            S.dma(eng, fn, R=Rk, W=[out])

        def allgather(out, in_):
            S.coll(lambda e: e.collective_compute("AllGather", ALU.bypass, replica_groups=[list(range(NCORE))],
                                                  ins=[in_.ap], outs=[out.ap]), R=[in_], W=[out])

        cm = sb(es_all, "cm", [128, 10, 128])
        cmb = sb(es_all, "cmb", [128, 10, 128], BF16)
        sel = sb(es_all, "sel", [32, 32, 128], BF16)
        mod = sb(es_all, "mod", [128, 2, 96, 5])
        lnp_s = sb(es_all, "lnp_s", [128, 2, 4, KC])
        ones_row = sb(es_all, "ones_row", [1, 128])
        ones_rowb = sb(es_all, "ones_rowb", [1, 128], BF16)
        dma(V(cm[:], "cm"), V(cmat[:, :, :], "cmat"))
        dma(V(cmb[:], "cmb"), V(cmat[:, :, :], "cmat"), eng="pool")
        dma(V(sel[:], "sel"), V(selm[:, :, :], "selm"), eng="pool")
        dma(V(lnp_s[:], "lnp"), V(lnp[:, :, :, :], "lnpd"))
        mset(V(ones_row[:], "ones_row"), 1.0)
        mset(V(ones_rowb[:], "ones_rowb"), 1.0)
        IDENT, TRIF, TRIB, M2F, M2B, ONES, SW128, SW64 = range(8)

        def C(i, p=128, n=128):
            return V(cm[0:p, i, 0:n], "cm")

        def Cb(i, p=128, n=128):
            return V(cmb[0:p, i, 0:n], "cmb")

        MODV = V(mod[:], "mod")

        def modv(l, which, kc, s):
            return V(mod[:, l, which * KC + kc, s:s + 1], "mod")

        def gather_rows(dst, tmp, src, key):
            rows = src.shape[0]
            for r0 in range(0, rows, 512):
                r1 = min(rows, r0 + 512)
                dma(V(tmp[r0:r1, :], key + "i"), V(src[r0:r1, :], key + "s"))
            allgather(V(dst[:, :], key), V(tmp[:, :], key + "i"))

        for l in range(2):
            gather_rows(ADA[l], ADAi[l], ada_sh[l], "ADA%d" % l)
        for h in range(2):
            gather_rows(XT[h], XTi[h], xT_sh[:, h * HT:(h + 1) * HT], "XT%d" % h)
        for l in range(2):
            gather_rows(WG[l], WGi[l], wg_sh[l], "WG%d" % l)
            gather_rows(WU[l], WUi[l], wu_sh[l], "WU%d" % l)
            gather_rows(WD[l], WDi[l], wd_sh[l], "WD%d" % l)

        with ExitStack() as es:
            cs = sb(es, "cs", [128, KC, 5])
            awt = sb(es, "awt", [128, KC, 768])
            abt = sb(es, "abt", [128, 2, 96])
            pm_ = psum(es, "pm")
            pm = pm_[:, 0:48].rearrange("p (o f) -> p o f", f=8)
            dma(V(cs[:], "cs"), V(cT[:, :, :], "cTd"))
            dma(V(abt[:], "abt"), V(ada_bT.rearrange("l p o -> p l o"), "abd"))
            act(V(cs[:], "cs"), V(cs[:], "cs"), AF.Silu)
            for l in range(2):
                for blk in range(16):
                    dma(V(awt[:], "awt"),
                        V(ADA[l][:, blk * 768:(blk + 1) * 768].rearrange("(k p) n -> p k n", p=128), "ADA%d" % l))
                    for o in range(6):
                        for k in range(KC):
                            mm(V(pm[:, o, 0:5], "pm"), V(awt[:, k, o * 128:(o + 1) * 128], "awt"), V(cs[:, k, :], "cs"),
                               start=(k == 0), stop=(k == KC - 1))
                    for o in range(6):
                        oc = blk * 6 + o
                        ts(V(mod[:, l, oc, :], "mod"), V(pm[:, o, 0:5], "pm"), V(abt[:, l, oc:oc + 1], "abt"), ALU.add)
                for which in (1, 2, 4, 5):
                    ts(V(mod[:, l, which * KC:(which + 1) * KC, :], "mod"), V(mod[:, l, which * KC:(which + 1) * KC, :], "mod"),
                       1.0, ALU.add)
            S.barrier()
            S.flush()

        def load_u_tile(l, s, t0, xs):
            xv = V(xs[:], "xs")
            if l == 0:
                g0 = base[s] + t0
                h = g0 // HT
                dma(xv, V(XT[h][:, g0 - h * HT:g0 - h * HT + TT].rearrange("(k p) n -> p k n", p=128), "XT%d" % h), eng="pool")
                for k in range(KC):
                    act(V(xs[:, k, :], "xs"), V(xs[:, k, :], "xs"), AF.Identity, bias=modv(0, 0, k, s), scale=modv(0, 1, k, s))
            else:
                o = own[s]
                t = t0
                while t < t0 + TT:
                    r = t // o
                    n = min(t0 + TT, (r + 1) * o) - t
                    c0 = obase[s] + (t - r * o)
                    dma(V(xs[:, :, t - t0:t - t0 + n], "xs"),
                        V(GU[r * D:(r + 1) * D, c0:c0 + n].rearrange("(k p) n -> p k n", p=128), "GU"))
                    t += n

        def proj_fm(ps, W, c0, M, xs, kcs=KC):
            for k in range(kcs):
                mm(ps, V(W[0][:, k, c0:c0 + M], W[1]), V(xs[:, k, :], "xs"), start=(k == 0), stop=(k == kcs - 1))

        def proj_tm(ps, W, c0, N, xs, blk, brow):
            for k in range(KC):
                mm(ps, V(xs[:, k, blk * 128:(blk + 1) * 128], "xs"), V(W[0][:, k, c0:c0 + N], W[1]), start=(k == 0), stop=False)
            mm(ps, V(ones_rowb[:], "ones_rowb"), brow, start=False, stop=True)

        def rstd_from_sum(out, ssum_ps, n, eps, tmp):
            ts(tmp, ssum_ps, float(n * eps), ALU.add)
            act(tmp, tmp, AF.Sqrt)
            recip(out, tmp)

        def attention(S_len, s, l, kparts, q_fn, Vaug, scale, mix, mixrow, es):
            pS = [psum(es, "pS%d" % i) for i in range(2)]
            pO = [psum(es, "pO%d" % i)[:, 0:258].rearrange("p (a b) -> p a b", b=129) for i in range(2)]
            pT = psum(es, "pTa", None, BF16)[:, 0:128]
            PT = [sb(es, "PT%d" % i, [128, TT], BF16) for i in range(3)]
            on = sb(es, "on", [128, 128], BF16)
            rc = sb(es, "rc", [128, 1])
            mt = sb(es, "mt", [128, TT], BF16)
            nkb = S_len // 128
            it = 0
            for t0 in range(0, S_len, TT):
                qs = q_fn(t0)
                for kb in range(nkb):
                    ps = pS[it % 2]
                    psn = "pS%d" % (it % 2)
                    pt = PT[it % 3]
                    ptn = "PT%d" % (it % 3)
                    it += 1
                    for pi, (KTt, npart, kkey) in enumerate(kparts):
                        mm(V(ps[:], psn), V(KTt[0:npart, kb * 128:(kb + 1) * 128], kkey), qs[pi],
                           start=(pi == 0), stop=(pi == len(kparts) - 1))
                    act(V(pt[:], ptn), V(ps[:], psn), AF.Exp, scale=scale)
                    for i in range(4):
                        mm(V(pO[i // 2][:, i % 2, :], "pO%d_%d" % (i // 2, i % 2)), V(pt[:, i * 128:(i + 1) * 128], ptn),
                           V(Vaug[:, kb, :], "Vaug"), start=(kb == 0), stop=(kb == nkb - 1))
                for i in range(4):
                    po = V(pO[i // 2][:, i % 2, :], "pO%d_%d" % (i // 2, i % 2))
                    recip(V(rc[:], "rc"), V(pO[i // 2][:, i % 2, 128:129], po.keys[0]))
                    ts(V(on[:], "on"), V(pO[i // 2][:, i % 2, 0:128], po.keys[0]), V(rc[:], "rc"), ALU.mult)
                    tr(V(pT[:], "pTa"), V(on[:], "on"), Cb(IDENT))
                    cp(V(mt[:, i * 128:(i + 1) * 128], "mt"), V(pT[:], "pTa"), eng="act")
                g0 = base[s] + t0
                dma(V(mix[mixrow:mixrow + 128, g0:g0 + TT], "mixd"), V(mt[:], "mt"))

        def lin_attn_chunk(fwd, ndk, qT, kT, k_tm, v_tm, NV, la, scale, S32, Sb, pz, tmp):
            tri = C(TRIF if fwd else TRIB)
            m2 = C(M2F if fwd else M2B)
            last = 127 if fwd else 0
            Ep, En, Ew, qt, kt, AT, kh = tmp
            mm(V(pz[0][:, 0:128], "pz0"), la, tri)
            act(V(Ep[:], "Ep"), V(pz[0][:, 0:128], "pz0"), AF.Exp)
            act(V(En[:], "En"), V(pz[0][:, 0:128], "pz0"), AF.Exp, scale=-1.0)
            for j in range(ndk):
                stt(V(qt[:, j, :], "qt"), qT[j], scale, V(Ep[:], "Ep"), ALU.mult, ALU.mult)
                tt(V(kt[:, j, :], "kt"), kT[j], V(En[:], "En"), ALU.mult)
            for j in range(ndk):
                mm(V(pz[1][:, 0:128], "pz1"), V(kt[:, j, :], "kt"), V(qt[:, j, :], "qt"), start=(j == 0), stop=(j == ndk - 1))
            tt(V(AT[:], "AT"), V(pz[1][:, 0:128], "pz1"), tri, ALU.mult)
            po = V(pz[2][:, 0:NV], "pz2")
            mm(po, V(AT[:], "AT"), v_tm, start=True, stop=False)
            for j in range(ndk):
                mm(po, V(qt[:, j, :], "qt"), V(Sb[:, j, 0:NV], "Sb"), start=False, stop=(j == ndk - 1))
            mm(V(pz[3][:, 0:128], "pz3"), m2, la)
            act(V(Ew[:], "Ew"), V(pz[3][:, 0:128], "pz3"), AF.Exp)
            for j in range(ndk):
                tt(V(kh[:, j, :], "kh"), V(k_tm.ap[:, j * 128:(j + 1) * 128], *k_tm.keys), V(Ew[:], "Ew"), ALU.mult)
            for j in range(ndk):
                pd = V(pz[4 + (j % 2)][:, 0:NV], "pz%d" % (4 + (j % 2)))
                mm(pd, V(kh[:, j, :], "kh"), v_tm)
                stt(V(S32[:, j, 0:NV], "S32"), V(S32[:, j, 0:NV], "S32"), V(Ep[:, last:last + 1], "Ep"), pd, ALU.mult, ALU.add)
                cp(V(Sb[:, j, 0:NV], "Sb"), V(S32[:, j, 0:NV], "S32"), eng="act")
            return po

        def logsig_neg(out, in_, tmp, sc_out):
            act(tmp, in_, AF.Exp, scale=-1.0)
            act(tmp, tmp, AF.Ln, bias=1.0, scale=1.0)
            ts(out, tmp, -sc_out, ALU.mult)

        def phase_A(l):
            with ExitStack() as es:
                ncol = 1156 if l == 0 else 1376
                Wt = sb(es, "Wt", [128, KC, ncol], BF16)
                W = (Wt, "Wt")
                wsrc = w0 if l == 0 else w1
                for k4 in range(4):
                    dma(V(Wt[:, 4 * k4:4 * k4 + 4, :], "Wt"),
                        V(wsrc[k4 * 512:(k4 + 1) * 512, :].rearrange("(k p) n -> p k n", p=128), "wsrc"), eng="pool")
                bfm = sb(es, "bfm", [128, 12])
                btm = sb(es, "btm", [1, ncol], BF16)
                if l == 0:
                    dma(V(bfm[:, 0:8], "bfm"), V(b0fm[:, :], "bd"))
                    dma(V(btm[:], "btm"), V(b0tm[:, :], "bd2"), eng="pool")
                    gn = sb(es, "gn", [128, 2])
                    dma(V(gn[:], "gn"), V(gqk[:, :], "gd"))
                else:
                    dma(V(bfm[:], "bfm"), V(b1fm[:, :], "bd"))
                    dma(V(btm[:], "btm"), V(b1tm[:, :], "bd2"), eng="pool")
                    gn = sb(es, "gn", [128, 6])
                    dma(V(gn[:], "gn"), V(gcq[:, :], "gd"))
                    wuq_s = sb(es, "wuq_s", [128, 4, 192], BF16)
                    wukv_s = sb(es, "wukv_s", [128, 2, 256], BF16)
                    wgla_s = sb(es, "wgla_s", [16, 2, 128], BF16)
                    bgla_s = sb(es, "bgla_s", [1, 2, 128], BF16)
                    dma(V(wuq_s[:], "wuq_s"), V(wuq.rearrange("(k p) n -> p k n", p=128), "wuqd"), eng="pool")
                    dma(V(wukv_s[:], "wukv_s"), V(wukv.rearrange("(k p) n -> p k n", p=128), "wukvd"), eng="pool")
                    dma(V(wgla_s[:], "wgla_s"), V(wgla[:, :, :], "wglad"), eng="pool")
                    dma(V(bgla_s[:], "bgla_s"), V(bgla[:, :, :], "bglad"), eng="pool")
                xs = sb(es, "xs", [128, KC, TT], BF16)
                pA = [psum(es, "pA%d" % i) for i in range(2)]
                f1 = sb(es, "f1", [128, TT])
                f2 = sb(es, "f2", [128, TT])
                f3 = sb(es, "f3", [128, TT])
                b1_ = sb(es, "b1_", [128, TT], BF16)
                cst = sb(es, "cst", [128, TT])
                snt = sb(es, "snt", [128, TT])
                if l == 0:
                    mix_attn, mix_lin, mix_silu = (mix0, 0), (mix0, 128), None
                else:
                    mix_attn, mix_lin, mix_silu = (mix1b, 0), (mix1a, 0), (mix1a, 128)

                def bias(j, p=128):
                    return V(bfm[0:p, j:j + 1], "bfm")

                def norm_rope(dst, src_ps, bj, gj, t0, np_, swapi, ctab, stab):
                    act(V(f1[0:np_, :], "f1"), src_ps, AF.Identity, bias=bias(bj, np_))
                    if gj is not None:
                        act(V(f2[0:np_, :], "f2"), V(f1[0:np_, :], "f1"), AF.Square)
                        mm(V(pA[1][0:np_, :], "pA1"), C(ONES, np_, np_), V(f2[0:np_, :], "f2"))
                        rstd_from_sum(V(f2[0:np_, :], "f2"), V(pA[1][0:np_, :], "pA1"), np_, RMS_EPS, V(f3[0:np_, :], "f3"))
                        stt(V(f1[0:np_, :], "f1"), V(f1[0:np_, :], "f1"), V(gn[0:np_, gj:gj + 1], "gn"), V(f2[0:np_, :], "f2"),
                            ALU.mult, ALU.mult)
                    cp(V(b1_[0:np_, :], "b1_"), V(f1[0:np_, :], "f1"), eng="act")
                    mm(V(pA[1][0:np_, :], "pA1"), Cb(swapi, np_, np_), V(b1_[0:np_, :], "b1_"))
                    dma(V(cst[0:np_, :], "cst"), V(ctab[0:np_, t0:t0 + TT], "ctab"))
                    dma(V(snt[0:np_, :], "snt"), V(stab[0:np_, t0:t0 + TT], "stab"))
                    tt(V(f2[0:np_, :], "f2"), V(f1[0:np_, :], "f1"), V(cst[0:np_, :], "cst"), ALU.mult)
                    tt(V(f3[0:np_, :], "f3"), V(pA[1][0:np_, :], "pA1"), V(snt[0:np_, :], "snt"), ALU.mult)
                    tt(dst, V(f2[0:np_, :], "f2"), V(f3[0:np_, :], "f3"), ALU.add)

                for s in range(5):
                    SL = lens[s]
                    nb = SL // 128
                    with ExitStack() as es2:
                        KT = sb(es2, "KT", [128, SL], BF16)
                        Vaug = sb(es2, "Vaug", [128, nb, 129], BF16)
                        mset(V(Vaug[:, :, 128:129], "Vaug"), 1.0)
                        qn = sb(es2, "qn", [128, TT], BF16)
                        pV = psum(es2, "pV")[:, 0:128]
                        if l == 1:
                            KR = sb(es2, "KR", [64, SL], BF16)
                            qr = sb(es2, "qr", [64, TT], BF16)
                            cqn = sb(es2, "cqn", [128, 4, TT], BF16)
                        for t0 in range(0, SL, TT):
                            load_u_tile(l, s, t0, xs)
                            if l == 0:
                                proj_fm(V(pA[0][:], "pA0"), W, 128, 128, xs)
                                norm_rope(V(KT[:, t0:t0 + TT], "KT"), V(pA[0][:], "pA0"), 1, 1, t0, 128, SW128, cos1, sin1)
                                for blk in range(4):
                                    proj_tm(V(pV[:], "pV"), W, 256, 128, xs, blk, V(btm[:, 256:384], "btm"))
                                    cp(V(Vaug[:, t0 // 128 + blk, 0:128], "Vaug"), V(pV[:], "pV"), eng="act")
                            else:
                                for j in range(2):
                                    proj_fm(V(pA[0][:], "pA0"), W, 1056 + j * 128, 128, xs)
                                    act(V(cqn[:, j, :], "cqn"), V(pA[0][:], "pA0"), AF.Identity, bias=bias(9 + j))
                                    act(V(f2[:], "f2"), V(pA[0][:], "pA0"), AF.Square, bias=bias(9 + j))
                                    mm(V(pA[1][:], "pA1"), C(ONES), V(f2[:], "f2"), start=(j == 0), stop=(j == 1))
                                rstd_from_sum(V(f2[:], "f2"), V(pA[1][:], "pA1"), 256, RMS_EPS, V(f3[:], "f3"))
                                for j in range(2):
                                    stt(V(cqn[:, j, :], "cqn"), V(cqn[:, j, :], "cqn"), V(gn[:, 4 + j:5 + j], "gn"), V(f2[:], "f2"),
                                        ALU.mult, ALU.mult)
                                for j in range(2):
                                    mm(V(pA[0][:], "pA0"), V(wukv_s[:, j, 0:128], "wukv_s"), V(cqn[:, j, :], "cqn"),
                                       start=(j == 0), stop=(j == 1))
                                cp(V(KT[:, t0:t0 + TT], "KT"), V(pA[0][:], "pA0"), eng="act")
                                for blk in range(4):
                                    for j in range(2):
                                        mm(V(pV[:], "pV"), V(cqn[:, j, blk * 128:(blk + 1) * 128], "cqn"), V(wukv_s[:, j, 128:256], "wukv_s"),
                                           start=(j == 0), stop=(j == 1))
                                    cp(V(Vaug[:, t0 // 128 + blk, 0:128], "Vaug"), V(pV[:], "pV"), eng="act")
                                proj_fm(V(pA[0][0:64, :], "pA0"), W, 1312, 64, xs)
                                norm_rope(V(KR[:, t0:t0 + TT], "KR"), V(pA[0][0:64, :], "pA0"), 11, None, t0, 64, SW64, cos2, sin2)

                        def q_fn(t0):
                            load_u_tile(l, s, t0, xs)
                            if l == 0:
                                proj_fm(V(pA[0][:], "pA0"), W, 0, 128, xs)
                                norm_rope(V(qn[:], "qn"), V(pA[0][:], "pA0"), 0, 0, t0, 128, SW128, cos1, sin1)
                                return [V(qn[:], "qn")]
                            for j in range(4):
                                proj_fm(V(pA[0][:], "pA0"), W, 544 + j * 128, 128, xs)
                                act(V(cqn[:, j, :], "cqn"), V(pA[0][:], "pA0"), AF.Identity, bias=bias(5 + j))
                                act(V(f2[:], "f2"), V(pA[0][:], "pA0"), AF.Square, bias=bias(5 + j))
                                mm(V(pA[1][:], "pA1"), C(ONES), V(f2[:], "f2"), start=(j == 0), stop=(j == 3))
                            rstd_from_sum(V(f2[:], "f2"), V(pA[1][:], "pA1"), 512, RMS_EPS, V(f3[:], "f3"))
                            for j in range(4):
                                stt(V(cqn[:, j, :], "cqn"), V(cqn[:, j, :], "cqn"), V(gn[:, j:j + 1], "gn"), V(f2[:], "f2"),
                                    ALU.mult, ALU.mult)
                            for j in range(4):
                                mm(V(pA[0][:], "pA0"), V(wuq_s[:, j, 0:128], "wuq_s"), V(cqn[:, j, :], "cqn"), start=(j == 0), stop=(j == 3))
                            cp(V(qn[:], "qn"), V(pA[0][:], "pA0"), eng="act")
                            for j in range(4):
                                mm(V(pA[0][0:64, :], "pA0"), V(wuq_s[:, j, 128:192], "wuq_s"), V(cqn[:, j, :], "cqn"),
                                   start=(j == 0), stop=(j == 3))
                            ts(V(f1[0:64, :], "f1"), V(pA[0][0:64, :], "pA0"), 0.0, ALU.add)
                            cp(V(b1_[0:64, :], "b1_"), V(f1[0:64, :], "f1"), eng="act")
                            mm(V(pA[1][0:64, :], "pA1"), Cb(SW64, 64, 64), V(b1_[0:64, :], "b1_"))
                            dma(V(cst[0:64, :], "cst"), V(cos2[0:64, t0:t0 + TT], "ctab"))
                            dma(V(snt[0:64, :], "snt"), V(sin2[0:64, t0:t0 + TT], "stab"))
                            tt(V(f2[0:64, :], "f2"), V(f1[0:64, :], "f1"), V(cst[0:64, :], "cst"), ALU.mult)
                            tt(V(f3[0:64, :], "f3"), V(pA[1][0:64, :], "pA1"), V(snt[0:64, :], "snt"), ALU.mult)
                            tt(V(qr[:], "qr"), V(f2[0:64, :], "f2"), V(f3[0:64, :], "f3"), ALU.add)
                            return [V(qn[:], "qn"), V(qr[:], "qr")]

                        if l == 0:
                            attention(SL, s, l, [(KT, 128, "KT")], q_fn, Vaug, 128 ** -0.5, mix_attn[0], mix_attn[1], es2)
                        else:
                            attention(SL, s, l, [(KT, 128, "KT"), (KR, 64, "KR")], q_fn, Vaug, 192 ** -0.5, mix_attn[0], mix_attn[1], es2)
                        S.barrier()
                        S.flush()

                    with ExitStack() as es2:
                        ndk = 2 if l == 0 else 1
                        NV = 129 if l == 0 else 128
                        HB = sb(es2, "HB", [128, nb, 128])
                        S32 = sb(es2, "S32", [128, ndk, 129])
                        Sb = sb(es2, "Sb", [128, ndk, 129], BF16)
                        pz = [psum(es2, "pz%d" % i) for i in range(6)]
                        tmp = (sb(es2, "Ep", [128, 128]), sb(es2, "En", [128, 128]), sb(es2, "Ew", [128, 128]),
                               sb(es2, "qt", [128, ndk, 128], BF16), sb(es2, "kt", [128, ndk, 128], BF16),
                               sb(es2, "AT", [128, 128], BF16), sb(es2, "kh", [128, ndk, 128], BF16))
                        qf = sb(es2, "qf", [128, ndk, TT])
                        kf = sb(es2, "kf", [128, ndk, TT])
                        ktm = sb(es2, "ktm", [128, 256])
                        vtm = sb(es2, "vtm", [128, 129], BF16)
                        gts = sb(es2, "gts", [128, 8])
                        la = sb(es2, "la", [128, 128])
                        eli = sb(es2, "eli", [128, 1])
                        hh = sb(es2, "hh", [128, 128])
                        dn = sb(es2, "dn", [128, 1])
                        gT = sb(es2, "gT", [128, TT])
                        lrT = sb(es2, "lrT", [16, TT], BF16)
                        mo = sb(es2, "mo", [128, TT], BF16)
                        mo2 = sb(es2, "mo2", [128, TT], BF16)
                        tiles = list(range(0, SL, TT))
                        for fwd in (True, False):
                            mset(V(S32[:], "S32"), 0.0)
                            mset(V(Sb[:], "Sb"), 0.0)
                            for t0 in (tiles if fwd else tiles[::-1]):
                                load_u_tile(l, s, t0, xs)
                                if l == 0:
                                    for j in range(2):
                                        proj_fm(V(pA[0][:], "pA0"), W, 384 + j * 128, 128, xs)
                                        act(V(qf[:, j, :], "qf"), V(pA[0][:], "pA0"), AF.Identity, bias=bias(2 + j))
                                        proj_fm(V(pA[1][:], "pA1"), W, 640 + j * 128, 128, xs)
                                        act(V(kf[:, j, :], "kf"), V(pA[1][:], "pA1"), AF.Identity, bias=bias(4 + j))
                                    if not fwd:
                                        proj_fm(V(pA[0][:], "pA0"), W, 1024, 128, xs)
                                        act(V(gT[:], "gT"), V(pA[0][:], "pA0"), AF.Sigmoid, bias=bias(6))
                                else:
                                    proj_fm(V(pA[0][:], "pA0"), W, 0, 128, xs)
                                    act(V(qf[:, 0, :], "qf"), V(pA[0][:], "pA0"), AF.Identity, bias=bias(0))
                                    proj_fm(V(pA[1][:], "pA1"), W, 128, 128, xs)
                                    act(V(kf[:, 0, :], "kf"), V(pA[1][:], "pA1"), AF.Identity, bias=bias(1))
                                    lc = 512 if fwd else 528
                                    proj_fm(V(pA[0][0:16, :], "pA0"), W, lc, 16, xs)
                                    act(V(lrT[:], "lrT"), V(pA[0][0:16, :], "pA0"), AF.Identity, bias=bias(3 if fwd else 4, 16))
                                    if not fwd:
                                        proj_fm(V(pA[0][:], "pA0"), W, 384, 128, xs)
                                        act(V(gT[:], "gT"), V(pA[0][:], "pA0"), AF.Silu, bias=bias(2))
                                        g0 = base[s] + t0
                                        cp(V(mo2[:], "mo2"), V(gT[:], "gT"))
                                        dma(V(mix_silu[0][mix_silu[1]:mix_silu[1] + 128, g0:g0 + TT], "mixd"), V(mo2[:], "mo2"))
                                for blk in (range(4) if fwd else range(3, -1, -1)):
                                    gb = t0 // 128 + blk
                                    cs_ = slice(blk * 128, (blk + 1) * 128)
                                    if l == 0:
                                        proj_tm(V(pz[4][:, 0:4], "pz4"), W, 1152, 4, xs, blk, V(btm[:, 1152:1156], "btm"))
                                        cp(V(gts[:, 0:4], "gts"), V(pz[4][:, 0:4], "pz4"))
                                        gi = 0 if fwd else 2
                                        logsig_neg(V(gts[:, 5:6], "gts"), V(gts[:, gi + 1:gi + 2], "gts"), V(gts[:, 4:5], "gts"), 1.0)
                                        cp(V(la[:], "la"), V(gts[:, 5:6].to_broadcast([128, 128]), "gts"))
                                        act(V(eli[:], "eli"), V(gts[:, gi:gi + 1], "gts"), AF.Exp)
                                        for j in range(2):
                                            proj_tm(V(pz[5][:, 0:128], "pz5"), W, 640 + j * 128, 128, xs, blk,
                                                    V(btm[:, 640 + j * 128:768 + j * 128], "btm"))
                                            cp(V(ktm[:, j * 128:(j + 1) * 128], "ktm"), V(pz[5][:, 0:128], "pz5"), eng="act")
                                        proj_tm(V(pz[5][:, 0:128], "pz5"), W, 896, 128, xs, blk, V(btm[:, 896:1024], "btm"))
                                        ts(V(vtm[:, 0:128], "vtm"), V(pz[5][:, 0:128], "pz5"), V(eli[:], "eli"), ALU.mult)
                                        cp(V(vtm[:, 128:129], "vtm"), V(eli[:], "eli"))
                                        sc_ = 256 ** -0.5
                                    else:
                                        gi = 0 if fwd else 1
                                        mm(V(pz[4][:, 0:128], "pz4"), V(lrT[:, cs_], "lrT"), V(wgla_s[:, gi, :], "wgla_s"), start=True, stop=False)
                                        mm(V(pz[4][:, 0:128], "pz4"), V(ones_rowb[:], "ones_rowb"), V(bgla_s[:, gi, :], "bgla_s"),
                                           start=False, stop=True)
                                        logsig_neg(V(la[:], "la"), V(pz[4][:, 0:128], "pz4"), V(hh[:], "hh"), 1.0 / 16.0)
                                        proj_tm(V(pz[5][:, 0:128], "pz5"), W, 128, 128, xs, blk, V(btm[:, 128:256], "btm"))
                                        cp(V(ktm[:, 0:128], "ktm"), V(pz[5][:, 0:128], "pz5"), eng="act")
                                        proj_tm(V(pz[5][:, 0:128], "pz5"), W, 256, 128, xs, blk, V(btm[:, 256:384], "btm"))
                                        cp(V(vtm[:, 0:128], "vtm"), V(pz[5][:, 0:128], "pz5"), eng="act")
                                        sc_ = 128 ** -0.5
                                    po = lin_attn_chunk(fwd, ndk, [V(qf[:, j, cs_], "qf") for j in range(ndk)],
                                                        [V(kf[:, j, cs_], "kf") for j in range(ndk)],
                                                        V(ktm[:, 0:ndk * 128], "ktm"), V(vtm[:, 0:NV], "vtm"), NV, V(la[:], "la"),
                                                        sc_, S32, Sb, pz, tmp)
                                    if l == 0:
                                        ts(V(dn[:], "dn"), V(pz[2][:, 128:129], "pz2"), -1.0, ALU.mult)
                                        tt(V(dn[:], "dn"), V(dn[:], "dn"), V(pz[2][:, 128:129], "pz2"), ALU.max)
                                        ts(V(dn[:], "dn"), V(dn[:], "dn"), 1.0, ALU.max)
                                        recip(V(dn[:], "dn"), V(dn[:], "dn"))
                                        src = V(hh[:], "hh")
                                        ts(src, V(pz[2][:, 0:128], "pz2"), V(dn[:], "dn"), ALU.mult)
                                    else:
                                        src = V(pz[2][:, 0:128], "pz2")
                                    hb = V(HB[:, gb, :], ("HB", gb))
                                    if fwd:
                                        cp(hb, src)
                                    else:
                                        tt(hb, hb, src, ALU.add)
                                        tr(V(pz[0][:, 0:128], "pz0"), hb, C(IDENT))
                                        if l == 0:
                                            tt(V(mo[:, cs_], "mo"), V(pz[0][:, 0:128], "pz0"), V(gT[:, cs_], "gT"), ALU.mult)
                                        else:
                                            cp(V(mo[:, cs_], "mo"), V(pz[0][:, 0:128], "pz0"))
                                if not fwd:
                                    g0 = base[s] + t0
                                    dma(V(mix_lin[0][mix_lin[1]:mix_lin[1] + 128, g0:g0 + TT], "mixd"), V(mo[:], "mo"))
                        S.barrier()
                        S.flush()

        def phase_B(l):
            GS = [G0] if l == 0 else [G1a, G1b]
            NCH = sum(g.shape[0] for g in GS) // 128
            with ExitStack() as es:
                xr = sb(es, "xr", [128, KC, TT])
                u2f = sb(es, "u2f", [128, KC, TT])
                u2b = sb(es, "u2b", [128, KC, TT], BF16)
                mixs = sb(es, "mixs", [128, NCH, TT], BF16)
                wo = [sb(es, "wo0", [128, KC, 256], BF16)] * 2
                wgs = sb(es, "wgs", [128, KC, 512], BF16)
                wus = sb(es, "wus", [128, KC, 512], BF16)
                wds = sb(es, "wds", [128, 4, D], BF16)
                wrs = sb(es, "wrs", [128, KC, 36])
                brs = sb(es, "brs", [1, 36])
                gmx = sb(es, "gmx", [128, 8])
                t1 = sb(es, "t1", [128, TT])
                t2 = sb(es, "t2", [128, TT])
                t3 = sb(es, "t3", [128, TT])
                hg = sb(es, "hg", [128, 4, TT], BF16)
                lg = sb(es, "lg", [128, 40])
                lw = sb(es, "lw", [128, 40])
                sm = sb(es, "sm", [128, 8])
                gates = sb(es, "gates", [128, 32])
                gTt = sb(es, "gTt", [32, TT], BF16)
                pB = [psum(es, "pB%d" % i) for i in range(8)]
                dma(V(wrs[:], "wrs"), V(wr[l].rearrange("(k p) n -> p k n", p=128), "wrd"))
                dma(V(brs[:], "brs"), V(br[l], "brd"))
                dma(V(gmx[:], "gmx"), V((gmix0 if l == 0 else gmix1)[:, :], "gmd"))
                wout = wout0 if l == 0 else wout1
                xsrc = x_own if l == 0 else x1own
                Gown = nc.dram_tensor("Gown%d" % l, [NCH * 128, TO], BF16).ap()
                PC = 512
                go = 0
                for G in GS:
                    for rp in range(0, G.shape[0], PC):
                        def mk(pid, rp=rp, G=G):
                            return G[rp:rp + PC, 0:4 * SP].rearrange("r (s t) -> r s t", s=4)[:, :, bass.ds(pid * own[0], own[0])]
                        dma_dyn(V(Gown[go + rp:go + rp + PC, 0:4 * own[0]].rearrange("r (s t) -> r s t", s=4), "Gown"), mk, [V(None, "G")])

                        def mk2(pid, rp=rp, G=G):
                            return G[rp:rp + PC, 4 * SP:][:, bass.ds(pid * own[4], own[4])]
                        dma_dyn(V(Gown[go + rp:go + rp + PC, 4 * own[0]:TO], "Gown"), mk2, [V(None, "G")])
                    go += G.shape[0]

                def segs(c0):
                    out = []
                    for s in range(5):
                        a = max(c0, obase[s])
                        b = min(c0 + TT, obase[s] + own[s])
                        if a < b:
                            out.append((s, a - c0, b - c0, a - obase[s]))
                    return out

                def layer_norm(src_keys, which):
                    for k in range(KC):
                        mm(V(pB[0][:], "pB0"), C(ONES), V(xr[:, k, :], "xr"), start=(k == 0), stop=(k == KC - 1))
                    for k in range(KC):
                        act(V(t1[:], "t1"), V(xr[:, k, :], "xr"), AF.Square)
                        mm(V(pB[1][:], "pB1"), C(ONES), V(t1[:], "t1"), start=(k == 0), stop=(k == KC - 1))
                    ts(V(t2[:], "t2"), V(pB[0][:], "pB0"), 1.0 / D, ALU.mult)
                    tt(V(t3[:], "t3"), V(t2[:], "t2"), V(t2[:], "t2"), ALU.mult)
                    stt(V(t3[:], "t3"), V(pB[1][:], "pB1"), 1.0 / D, V(t3[:], "t3"), ALU.mult, ALU.subtract)
                    ts(V(t3[:], "t3"), V(t3[:], "t3"), LN_EPS, ALU.add)
                    act(V(t3[:], "t3"), V(t3[:], "t3"), AF.Sqrt)
                    recip(V(t3[:], "t3"), V(t3[:], "t3"))
                    for k in range(KC):
                        tt(V(xr[:, k, :], "xr"), V(xr[:, k, :], "xr"), V(t2[:], "t2"), ALU.subtract)
                        tt(V(xr[:, k, :], "xr"), V(xr[:, k, :], "xr"), V(t3[:], "t3"), ALU.mult)
                        act(V(xr[:, k, :], "xr"), V(xr[:, k, :], "xr"), AF.Identity,
                            bias=V(lnp_s[:, l, which + 1, k:k + 1], "lnp"), scale=V(lnp_s[:, l, which, k:k + 1], "lnp"))

                for c0 in range(0, TO, TT):
                    sg = segs(c0)
                    dma(V(xr[:], "xr"), V(xsrc[:, c0:c0 + TT].rearrange("(k p) n -> p k n", p=128), "xsrc"))
                    dma(V(mixs[:], "mixs"), V(Gown[:, c0:c0 + TT].rearrange("(k p) n -> p k n", p=128), "Gown"))
                    for hd in range(4):
                        chs = [(2 * hd + i) * 2 + (1 if l == 0 else 0) for i in range(2)]
                        for i, ch in enumerate(chs):
                            cp(V(t1[:], "t1"), V(mixs[:, ch, :], "mixs"))
                            tt(V(t1[:], "t1"), V(t1[:], "t1"), V(t1[:], "t1"), ALU.mult)
                            mm(V(pB[0][:], "pB0"), C(ONES), V(t1[:], "t1"), start=(i == 0), stop=(i == 1))
                        rstd_from_sum(V(t2[:], "t2"), V(pB[0][:], "pB0"), 256, RMS_EPS, V(t3[:], "t3"))
                        ts(V(t2[:], "t2"), V(t2[:], "t2"), 1.0, ALU.mult)
                        for i, ch in enumerate(chs):
                            r = 2 * hd + i
                            stt(V(t1[:], "t1"), V(mixs[:, ch, :], "mixs"), V(gmx[:, r:r + 1], "gmx"), V(t2[:], "t2"), ALU.mult, ALU.mult)
                            if l == 1:
                                tt(V(mixs[:, ch, :], "mixs"), V(t1[:], "t1"), V(mixs[:, ch + 1, :], "mixs"), ALU.mult)
                            else:
                                cp(V(mixs[:, ch, :], "mixs"), V(t1[:], "t1"))
                    if l == 0:
                        cch = list(range(16))
                    else:
                        cch = [x for r in range(NCORE) for x in (2 * r, 16 + r)]
                    act(V(xr[:], "xr"), V(xr[:], "xr"), AF.Identity, scale=ALPHA)
                    for nb_ in range(8):
                        wt = wo[nb_ % 2]
                        wtn = "wo0"
                        dma(V(wt[:], wtn), V(wout[:, nb_ * 256:(nb_ + 1) * 256].rearrange("(k p) n -> p k n", p=128), "woutd"),
                            eng="pool")
                        for dj in range(2):
                            dc = nb_ * 2 + dj
                            ps = pB[2 + dc % 2]
                            psn = "pB%d" % (2 + dc % 2)
                            for i, ch in enumerate(cch):
                                mm(V(ps[:], psn), V(wt[:, i, dj * 128:(dj + 1) * 128], wtn), V(mixs[:, ch, :], "mixs"),
                                   start=(i == 0), stop=(i == len(cch) - 1))
                            for (s, a, b, lo) in sg:
                                stt(V(xr[:, dc, a:b], "xr"), V(ps[:, a:b], psn), modv(l, 2, dc, s), V(xr[:, dc, a:b], "xr"),
                                    ALU.mult, ALU.add)
                    layer_norm(None, 0)
                    for k in range(KC):
                        for (s, a, b, lo) in sg:
                            act(V(u2f[:, k, a:b], "u2f"), V(xr[:, k, a:b], "xr"), AF.Identity, bias=modv(l, 3, k, s), scale=modv(l, 4, k, s))
                        cp(V(u2b[:, k, :], "u2b"), V(u2f[:, k, :], "u2f"), eng="pool")
                    for blk in range(4):
                        pr = V(pB[0][:, 0:36], "pB0")
                        for k in range(KC):
                            mm(pr, V(u2f[:, k, blk * 128:(blk + 1) * 128], "u2f"), V(wrs[:, k, :], "wrs"), start=(k == 0), stop=False)
                        mm(pr, V(ones_row[:], "ones_row"), V(brs[:], "brs"), start=False, stop=True)
                        LG = V(lg[:, 0:36], "lg")
                        cp(LG, pr)
                        rmax(V(sm[:, 0:1], "sm"), V(lg[:, 0:4], "lg"))
                        ts(V(lw[:, 0:4], "lw"), V(lg[:, 0:4], "lg"), V(sm[:, 0:1], "sm"), ALU.subtract)
                        act(V(lw[:, 4:8], "lw"), V(lw[:, 0:4], "lw"), AF.Exp)
                        rsum(V(sm[:, 1:2], "sm"), V(lw[:, 4:8], "lw"))
                        recip(V(sm[:, 1:2], "sm"), V(sm[:, 1:2], "sm"))
                        ts(V(lw[:, 0:4], "lw"), V(lw[:, 0:4], "lw"), 0.0, ALU.is_ge, NEG * -1.0, ALU.mult)
                        ts(V(lw[:, 0:4], "lw"), V(lw[:, 0:4], "lw"), NEG, ALU.add)
                        for g in range(4):
                            ts(V(lw[:, 8 + g * 8:16 + g * 8], "lw"), V(lg[:, 4 + g * 8:12 + g * 8], "lg"), V(lw[:, g:g + 1], "lw"), ALU.add)
                        LM = V(lw[:, 8:40], "lw")
                        rmax(V(sm[:, 2:3], "sm"), LM)
                        ts(V(gates[:], "gates"), LM, V(sm[:, 2:3], "sm"), ALU.is_ge)
                        stt(V(lg[:, 4:36], "lg"), V(gates[:], "gates"), NEG, LM, ALU.mult, ALU.add)
                        rmax(V(sm[:, 3:4], "sm"), V(lg[:, 4:36], "lg"))
                        ts(V(lw[:, 8:40], "lw"), V(lg[:, 4:36], "lg"), V(sm[:, 3:4], "sm"), ALU.is_ge)
                        tt(V(sm[:, 4:5], "sm"), V(sm[:, 3:4], "sm"), V(sm[:, 2:3], "sm"), ALU.subtract)
                        act(V(sm[:, 4:5], "sm"), V(sm[:, 4:5], "sm"), AF.Exp)
                        ts(V(sm[:, 5:6], "sm"), V(sm[:, 4:5], "sm"), 1.0, ALU.add)
                        recip(V(sm[:, 5:6], "sm"), V(sm[:, 5:6], "sm"))
                        tt(V(sm[:, 5:6], "sm"), V(sm[:, 5:6], "sm"), V(sm[:, 1:2], "sm"), ALU.mult)
                        tt(V(sm[:, 6:7], "sm"), V(sm[:, 5:6], "sm"), V(sm[:, 4:5], "sm"), ALU.mult)
                        ts(V(gates[:], "gates"), V(gates[:], "gates"), V(sm[:, 5:6], "sm"), ALU.mult)
                        stt(V(gates[:], "gates"), V(lw[:, 8:40], "lw"), V(sm[:, 6:7], "sm"), V(gates[:], "gates"), ALU.mult, ALU.add)
                        tr(V(pB[1][0:32, 0:128], "pB1"), V(gates[:], "gates"), C(IDENT))
                        cp(V(gTt[:, blk * 128:(blk + 1) * 128], "gTt"), V(pB[1][0:32, 0:128], "pB1"))
                    for e_ in range(32):
                        dma(V(wgs[:], "wgs"), V(WG[l][e_ * D:(e_ + 1) * D, :].rearrange("(k p) n -> p k n", p=128), "WG%d" % l), eng="pool")
                        dma(V(wus[:], "wus"), V(WU[l][e_ * D:(e_ + 1) * D, :].rearrange("(k p) n -> p k n", p=128), "WU%d" % l), eng="pool")
                        dma(V(wds[:], "wds"), V(WD[l][e_ * 512:(e_ + 1) * 512, :].rearrange("(k p) n -> p k n", p=128), "WD%d" % l), eng="pool")
                        mm(V(pB[6][:], "pB6"), V(sel[:, e_, :], "sel"), V(gTt[:], "gTt"))
                        cp(V(t3[:], "t3"), V(pB[6][:], "pB6"), eng="act")
                        for fc in range(4):
                            for k in range(KC):
                                mm(V(pB[4][:], "pB4"), V(wgs[:, k, fc * 128:(fc + 1) * 128], "wgs"), V(u2b[:, k, :], "u2b"),
                                   start=(k == 0), stop=(k == KC - 1))
                            for k in range(KC):
                                mm(V(pB[5][:], "pB5"), V(wus[:, k, fc * 128:(fc + 1) * 128], "wus"), V(u2b[:, k, :], "u2b"),
                                   start=(k == 0), stop=(k == KC - 1))
                            act(V(t1[:], "t1"), V(pB[4][:], "pB4"), AF.Silu)
                            tt(V(t2[:], "t2"), V(pB[5][:], "pB5"), V(t3[:], "t3"), ALU.mult)
                            tt(V(hg[:, fc, :], "hg"), V(t1[:], "t1"), V(t2[:], "t2"), ALU.mult)
                        for dc in range(KC):
                            ps = pB[2 + dc % 2]
                            psn = "pB%d" % (2 + dc % 2)
                            for fc in range(4):
                                mm(V(ps[:], psn), V(wds[:, fc, dc * 128:(dc + 1) * 128], "wds"), V(hg[:, fc, :], "hg"),
                                   start=(fc == 0), stop=(fc == 3))
                            if e_ == 0:
                                cp(V(u2f[:, dc, :], "u2f"), V(ps[:], psn), eng="act")
                            else:
                                tt(V(u2f[:, dc, :], "u2f"), V(u2f[:, dc, :], "u2f"), V(ps[:], psn), ALU.add)
                    for k in range(KC):
                        for (s, a, b, lo) in sg:
                            ts(V(u2f[:, k, a:b], "u2f"), V(u2f[:, k, a:b], "u2f"), modv(l, 5, k, s), ALU.mult)
                        stt(V(xr[:, k, :], "xr"), V(xr[:, k, :], "xr"), ALPHA, V(u2f[:, k, :], "u2f"), ALU.mult, ALU.add)
                    layer_norm(None, 2)
                    if l == 0:
                        dma(V(x1own[:, c0:c0 + TT].rearrange("(k p) n -> p k n", p=128), "x1own"), V(xr[:], "xr"))
                        for k in range(KC):
                            for (s, a, b, lo) in sg:
                                act(V(u2b[:, k, a:b], "u2b"), V(xr[:, k, a:b], "xr"), AF.Identity, bias=modv(1, 0, k, s),
                                    scale=modv(1, 1, k, s))
                        dma(V(u1own[:, c0:c0 + TT].rearrange("(k p) n -> p k n", p=128), "u1own"), V(u2b[:], "u2b"))
                    else:
                        dma(V(y_own[:, c0:c0 + TT].rearrange("(k p) n -> p k n", p=128), "y_own"), V(xr[:], "xr"))
                S.barrier()
                S.flush()

        phase_A(0)
        allgather(V(G0[:, :], "G"), V(mix0[:, :], "mixd"))
        S.barrier()
        S.flush()
        phase_B(0)
        allgather(V(GU[:, :], "GU"), V(u1own[:, :], "u1own"))
        S.barrier()
        S.flush()
        phase_A(1)
        allgather(V(G1a[:, :], "G"), V(mix1a[:, :], "mixd"))
        allgather(V(G1b[:, :], "G"), V(mix1b[:, :], "mixd"))
        S.barrier()
        S.flush()
        phase_B(1)
    return nc


def rope_tables(m, S):
    half = m // 2
    row = np.repeat(np.arange(S // 64), 64).astype(np.float32)
    col = np.tile(np.arange(64), S // 64).astype(np.float32)
    cos = np.zeros((m, S), np.float32)
    sin = np.zeros((m, S), np.float32)
    q = half // 2
    inv = (10000.0 ** (-np.arange(0, half, 2, dtype=np.float32) / half)).astype(np.float32)
    for bi, pos in enumerate((row, col)):
        ang = pos[None, :] * inv[:, None]
        c, s_ = np.cos(ang), np.sin(ang)
        o = bi * half
        cos[o:o + q] = c
        cos[o + q:o + 2 * q] = c
        sin[o:o + q] = -s_
        sin[o + q:o + 2 * q] = s_
    return cos, sin


def swap_mat(m):
    half = m // 2
    q = half // 2
    P = np.zeros((128, 128), np.float32)
    for i in range(m):
        blk, r = divmod(i, half)
        partner = blk * half + (r + q) % half
        P[partner, i] = 1.0
    return P


def consts():
    idx = np.arange(128)
    cm = np.zeros((128, 10, 128), np.float32)
    cm[:, 0] = np.eye(128)
    cm[:, 1] = (idx[:, None] <= idx[None, :])
    cm[:, 2] = (idx[:, None] >= idx[None, :])
    cm[:, 3] = (idx[:, None] > idx[None, :])
    cm[:, 4] = (idx[:, None] < idx[None, :])
    cm[:, 5] = 1.0
    cm[:, 6] = swap_mat(128)
    cm[:, 7] = swap_mat(64)
    sel = np.zeros((32, 32, 128), np.float32)
    for e in range(32):
        sel[e, e, :] = 1.0
    return cm, sel


def fm(vec, n=None):
    v = np.asarray(vec, np.float32).reshape(-1, 128)
    return np.ascontiguousarray(v.T)


def prepare(inputs, SP):
    lens, base, own, obase = seq_info(SP)
    f = lambda a: np.asarray(a, np.float32)
    xp, xsmp = f(inputs["x_prompt"]), f(inputs["x_sample"])
    toks = np.concatenate([xp.reshape(-1, D), xsmp.reshape(-1, D)], axis=0)
    xT = np.ascontiguousarray(toks.T)
    c_all = np.concatenate([f(inputs["c_prompt"]), f(inputs["c_sample"])], axis=0)
    cT = np.ascontiguousarray(c_all.T.reshape(KC, 128, 5).transpose(1, 0, 2))
    ada_bT = np.ascontiguousarray(f(inputs["ada_b"]).reshape(2, 96, 128).transpose(0, 2, 1))
    lnp = np.stack([np.stack([fm(inputs[k][l]) for k in ("ln1_g", "ln1_b", "ln2_g", "ln2_b")], axis=1) for l in range(2)], axis=1)
    cm, sel = consts()
    SMAX = lens[4]
    cos1, sin1 = rope_tables(128, SMAX)
    cos2, sin2 = rope_tables(64, SMAX)
    w_in0, b_in0 = f(inputs["ab_w_in"][0]), f(inputs["ab_b_in"][0])
    w_in1, b_in1 = f(inputs["cd_w_in"][0]), f(inputs["cd_b_in"][0])
    wout0_full, wout1_full = f(inputs["ab_w_out"][0]), f(inputs["cd_w_out"][0])
    perm0 = np.concatenate([np.concatenate([np.arange(r * 128, r * 128 + 128), 1024 + np.arange(r * 128, r * 128 + 128)])
                            for r in range(NCORE)])
    perm1 = np.concatenate([np.concatenate([np.arange(r * 128, r * 128 + 128), 1024 + np.arange(r * 128, r * 128 + 128)])
                            for r in range(NCORE)])
    common = dict(
        cT=cT, ada_bT=ada_bT, lnp=np.ascontiguousarray(lnp),
        wout0=np.ascontiguousarray(wout0_full[perm0]), wout1=np.ascontiguousarray(wout1_full[perm1]),
        wr=np.ascontiguousarray(np.concatenate([f(inputs["moe_w_rg"]), f(inputs["moe_w_re"])], axis=2)),
        br=np.ascontiguousarray(np.concatenate([f(inputs["moe_b_rg"]), f(inputs["moe_b_re"])], axis=1)[:, None, :]),
        cos1=cos1, sin1=sin1, cos2=cos2, sin2=sin2, cmat=cm, selm=sel,
        wgla=np.ascontiguousarray(np.stack([f(inputs["cd_w_gla_f"][0]), f(inputs["cd_w_gla_b"][0])], axis=1)),
    )
    g_ml = f(inputs["ab_g_mlstm"][0]) * np.sqrt(256.0)
    g_gl = f(inputs["cd_g_gla"][0]) * np.sqrt(256.0)
    common["gmix0"] = fm(g_ml)
    common["gmix1"] = fm(g_gl)
    maps = []
    for c in range(NCORE):
        kv, hb, hh = c // 4, c // 2, c % 2
        cols0 = np.concatenate([
            np.arange(c * 128, c * 128 + 128), 1024 + np.arange(kv * 128, kv * 128 + 128), 1280 + np.arange(kv * 128, kv * 128 + 128),
            1536 + np.arange(hb * 256, hb * 256 + 256), 2560 + np.arange(hb * 256, hb * 256 + 256),
            3584 + hb * 256 + hh * 128 + np.arange(128), 4608 + hb * 256 + hh * 128 + np.arange(128),
            5632 + np.array([hb, 4 + hb, 8 + hb, 12 + hb])])
        cols1 = np.concatenate([
            np.arange(hb * 128, hb * 128 + 128), 512 + np.arange(hb * 128, hb * 128 + 128),
            1024 + hb * 256 + hh * 128 + np.arange(128), 2048 + hb * 256 + hh * 128 + np.arange(128),
            3072 + np.arange(16), 3088 + np.arange(16), 3104 + np.arange(512), 3616 + np.arange(256), 3872 + np.arange(64)])
        b0 = b_in0[cols0]
        b1 = b_in1[cols1]
        b0fm = np.zeros((128, 8), np.float32)
        for j, o in enumerate((0, 128, 384, 512, 640, 768, 1024)):
            b0fm[:, j] = b0[o:o + 128]
        b1fm = np.zeros((128, 12), np.float32)
        for j, o in enumerate((0, 128, 384)):
            b1fm[:, j] = b1[o:o + 128]
        b1fm[0:16, 3] = b1[512:528]
        b1fm[0:16, 4] = b1[528:544]
        for j in range(4):
            b1fm[:, 5 + j] = b1[544 + j * 128:544 + (j + 1) * 128]
        for j in range(2):
            b1fm[:, 9 + j] = b1[1056 + j * 128:1056 + (j + 1) * 128]
        b1fm[0:64, 11] = b1[1312:1376]
        gqk = np.stack([f(inputs["ab_g_q"][0]) * np.sqrt(128.0), f(inputs["ab_g_k"][0]) * np.sqrt(128.0)], axis=1)
        gcq = np.concatenate([fm(f(inputs["cd_g_cq"][0]) * np.sqrt(512.0)), fm(f(inputs["cd_g_ckv"][0]) * np.sqrt(256.0))], axis=1)
        own_cols = np.concatenate([base[s] + c * own[s] + np.arange(own[s]) for s in range(5)])
        hs = slice(hb * 128, hb * 128 + 128)
        m = dict(common)
        RS = D // NCORE
        m.update(
            xT_sh=np.ascontiguousarray(xT[c * RS:(c + 1) * RS]),
            ada_sh=np.ascontiguousarray(f(inputs["ada_w"])[:, c * RS:(c + 1) * RS]),
            wg_sh=np.ascontiguousarray(f(inputs["moe_w_gate"])[:, 4 * c:4 * c + 4].reshape(2, 4 * D, 512)),
            wu_sh=np.ascontiguousarray(f(inputs["moe_w_up"])[:, 4 * c:4 * c + 4].reshape(2, 4 * D, 512)),
            wd_sh=np.ascontiguousarray(f(inputs["moe_w_down"])[:, 4 * c:4 * c + 4].reshape(2, 4 * 512, D)),
            x_own=np.ascontiguousarray(xT[:, own_cols]),
            w0=np.ascontiguousarray(w_in0[:, cols0]), b0fm=b0fm, b0tm=np.ascontiguousarray(b0[None, :]), gqk=np.ascontiguousarray(gqk),
            w1=np.ascontiguousarray(w_in1[:, cols1]), b1fm=b1fm, b1tm=np.ascontiguousarray(b1[None, :]),
            wgla=np.ascontiguousarray(np.stack([f(inputs["cd_w_gla_f"][0])[:, hs], f(inputs["cd_w_gla_b"][0])[:, hs]], axis=1)),
            bgla=np.ascontiguousarray(np.stack([f(inputs["cd_b_gla_f"][0])[hs], f(inputs["cd_b_gla_b"][0])[hs]], axis=0)[None]),
            gcq=np.ascontiguousarray(gcq),
            wuq=np.ascontiguousarray(f(inputs["cd_w_uq"][0])[:, c * 192:(c + 1) * 192]),
            wukv=np.ascontiguousarray(f(inputs["cd_w_ukv"][0])[:, c * 256:(c + 1) * 256]),
        )
        maps.append(m)
    return maps


def assemble(results, SP):
    lens, base, own, obase = seq_info(SP)
    TOT = sum(lens)
    yT = np.zeros((D, TOT), np.float32)
    for c in range(NCORE):
        yo = np.asarray(results[c]["y_own"], np.float32)
        for s in range(5):
            yT[:, base[s] + c * own[s]: base[s] + (c + 1) * own[s]] = yo[:, obase[s]:obase[s] + own[s]]
    y = np.ascontiguousarray(yT.T)
    return y[:4 * SP].reshape(4, SP, D), y[4 * SP:].reshape(1, 2 * SP, D)


def run(inputs, SP):
    nc = build_nc(SP)
    maps = prepare(inputs, SP)
    res = run_bass_kernel_spmd(nc, maps, core_ids=list(range(NCORE)))
    return assemble(res.results, SP)


def kernel(**inputs):
    SP = int(np.asarray(inputs["x_prompt"]).shape[1])
    yp, ys = run(inputs, SP)
    return yp.astype(np.float32), ys.astype(np.float32)
```
